# Optimizing a Trainium2 kernel written in Bass

```python
import math
import jax, jax.numpy as jnp
from jax import lax
import numpy as np

D_MODEL = 1024
BATCH = 2
SEQ = 16384
DEPTH = 4
DEC_BATCH = 16
DEC_SEQ = 4096
PAST_LEN = 128

POOL_GROUPS = 4
POOL_GROUP_DIM = 64
POOL_WIDTH = POOL_GROUPS * POOL_GROUP_DIM
POOL_WINDOWS = (2, 4, 8, 16)

HGRN_HEADS = 4
HGRN_HEAD_DIM = 64
HGRN_WIDTH = HGRN_HEADS * HGRN_HEAD_DIM
HGRN_CHUNK = 64

DIFF_HEADS = 4
DIFF_HEAD_DIM = 64
DIFF_V_DIM = 2 * DIFF_HEAD_DIM
DIFF_QK_WIDTH = DIFF_HEADS * 2 * DIFF_HEAD_DIM
DIFF_WIDTH = DIFF_HEADS * DIFF_V_DIM
ATTN_BLOCK = 128

D_MIX = POOL_WIDTH + HGRN_WIDTH + DIFF_WIDTH
IN_WIDTHS = (POOL_WIDTH,) + (HGRN_WIDTH,) * 5 + (DIFF_QK_WIDTH, DIFF_QK_WIDTH, DIFF_WIDTH)
IN_WIDTH = sum(IN_WIDTHS)
SPLIT_POINTS = tuple(sum(IN_WIDTHS[:i + 1]) for i in range(len(IN_WIDTHS) - 1))

D_FF = 2816
N_EXPERTS = 8
TOP_K = 2
D_FF_EXPERT = 2816
N_DENSE = (DEPTH + 1) // 2
N_MOE = DEPTH // 2

RMS_EPS = 1e-6

kernel_name = 'hybrid_pool_hgrn2_diffattn_encoder'


def _rmsnorm(x, g):
    xf = x.astype(jnp.float32)
    y = xf * lax.rsqrt(jnp.mean(xf * xf, axis=-1, keepdims=True) + RMS_EPS)
    return (y * g.astype(jnp.float32)).astype(x.dtype)


def _alibi_slopes(n):
    return jnp.asarray([2.0 ** (-8.0 * (h + 1) / n) for h in range(n)], dtype=jnp.float32)


def _pool_mixer(u, w_pool, scale):
    B, S, _ = u.shape
    uf = u.astype(jnp.float32)
    cs = jnp.concatenate([jnp.zeros((B, 1, POOL_WIDTH), jnp.float32), jnp.cumsum(uf, axis=1)], axis=1)
    t = jnp.arange(S)
    outs = []
    for gi, w in enumerate(POOL_WINDOWS):
        lo = jnp.clip(t - w // 2, 0, S)
        hi = jnp.clip(t + w // 2, 0, S)
        sl = slice(gi * POOL_GROUP_DIM, (gi + 1) * POOL_GROUP_DIM)
        csg = cs[:, :, sl]
        mean = (jnp.take(csg, hi, axis=1) - jnp.take(csg, lo, axis=1)) / (hi - lo).astype(jnp.float32)[None, :, None]
        outs.append(mean - uf[:, :, sl])
    d = jnp.stack(outs, axis=2).astype(u.dtype)
    y = jnp.einsum('bsgc,gcd->bsgd', d, w_pool).reshape(B, S, POOL_WIDTH)
    return y * scale


def _gla_chunk_scan(q, k, v, g):
    B, S, H, dk = q.shape
    dv = v.shape[-1]
    C = HGRN_CHUNK
    N = S // C

    def to_chunks(a):
        return a.reshape(B, N, C, H, a.shape[-1]).transpose(1, 0, 3, 2, 4)

    mask = jnp.tril(jnp.ones((C, C), dtype=bool))[None, None, :, :, None]

    def step(st, xs):
        qc, kc, vc, gc = xs
        b = jnp.cumsum(gc, axis=2)
        b_last = b[:, :, -1:, :]
        o_inter = jnp.einsum('bhtd,bhde->bhte', qc * jnp.exp(b), st)
        rel = jnp.where(mask, b[:, :, :, None, :] - b[:, :, None, :, :], 0.0)
        decay = jnp.where(mask, jnp.exp(rel), 0.0)
        A = jnp.einsum('bhtsd,bhsd->bhts', qc[:, :, :, None, :] * decay, kc)
        o = o_inter + jnp.einsum('bhts,bhse->bhte', A, vc)
        st_new = jnp.exp(b_last[:, :, 0, :])[..., None] * st + jnp.einsum('bhsd,bhse->bhde', kc * jnp.exp(b_last - b), vc)
        return st_new, o

    st0 = jnp.zeros((B, H, dk, dv), jnp.float32)
    _, o = lax.scan(step, st0, (to_chunks(q), to_chunks(k), to_chunks(v), to_chunks(g)))
    return o.transpose(1, 0, 3, 2, 4).reshape(B, S, H, dv)


def _hgrn2_mixer(q, fz_f, fz_b, i, gate, lb_f, lb_b, norm_g):
    B, S, _ = q.shape

    def heads(a):
        return a.astype(jnp.float32).reshape(B, S, HGRN_HEADS, HGRN_HEAD_DIM)

    qh = jax.nn.silu(heads(q))
    vh = heads(i)

    def decay(z, lb):
        z = heads(z)
        lb = lb.reshape(HGRN_HEADS, HGRN_HEAD_DIM)
        f = lb + (1.0 - lb) * jax.nn.sigmoid(z)
        logf = jnp.log(jnp.maximum(f, 1e-30))
        kk = (1.0 - lb) * jax.nn.sigmoid(-z)
        return logf, kk

    g_f, k_f = decay(fz_f, lb_f)
    g_b, k_b = decay(fz_b, lb_b)
    flip = lambda a: a[:, ::-1]
    o_f = _gla_chunk_scan(qh, k_f, vh, g_f)
    o_b = flip(_gla_chunk_scan(flip(qh), flip(k_b), flip(vh), flip(g_b)))
    o = o_f + o_b
    o = o * lax.rsqrt(jnp.mean(o * o, axis=-1, keepdims=True) + RMS_EPS) * norm_g.astype(jnp.float32)
    o = o.reshape(B, S, HGRN_WIDTH) * jax.nn.silu(gate.astype(jnp.float32))
    return o.astype(q.dtype)


def _diff_attention(q, k, v, lam, lam_init, norm_g):
    B, S, _ = q.shape
    H, d = DIFF_HEADS, DIFF_HEAD_DIM
    qh = q.reshape(B, S, H, 2, d).transpose(0, 2, 3, 1, 4) * (d ** -0.5)
    kh = k.reshape(B, S, H, 2, d).transpose(0, 2, 3, 1, 4)
    vf = v.reshape(B, S, H, DIFF_V_DIM).transpose(0, 2, 1, 3).astype(jnp.float32)
    nb = S // ATTN_BLOCK
    q_blocks = qh.reshape(B, H, 2, nb, ATTN_BLOCK, d).transpose(3, 0, 1, 2, 4, 5)
    slopes = _alibi_slopes(H)
    tk = jnp.arange(S)

    def block(xs):
        qb, start = xs
        tq = start + jnp.arange(ATTN_BLOCK)
        dist = jnp.abs(tq[:, None] - tk[None, :]).astype(jnp.float32)
        bias = -slopes[:, None, None] * dist
        s = jnp.einsum('bhmqd,bhmkd->bhmqk', qb, kh).astype(jnp.float32) + bias[None, :, None]
        p = jax.nn.softmax(s, axis=-1)
        a = p[:, :, 0] - lam * p[:, :, 1]
        return jnp.einsum('bhqk,bhkd->bhqd', a, vf)

    starts = jnp.arange(nb, dtype=jnp.int32) * ATTN_BLOCK
    o = lax.map(block, (q_blocks, starts))
    o = o.transpose(1, 0, 3, 2, 4).reshape(B, S, H, DIFF_V_DIM)
    o = o * lax.rsqrt(jnp.mean(o * o, axis=-1, keepdims=True) + RMS_EPS) * norm_g.astype(jnp.float32)
    o = o * (1.0 - lam_init)
    return o.reshape(B, S, DIFF_WIDTH).astype(q.dtype)


def _swiglu(x, w_gate, w_up, w_down):
    return (jax.nn.silu(x @ w_gate) * (x @ w_up)) @ w_down


def _moe_ffn(h, router_w, router_b, w_gate, w_up, w_down):
    B, S, D = h.shape
    t = h.reshape(B * S, D)
    logits = (t @ router_w).astype(jnp.float32) + router_b.astype(jnp.float32)
    top_v, top_i = lax.top_k(logits, TOP_K)
    probs = jax.nn.softmax(top_v, axis=-1)
    combine = jnp.sum(jax.nn.one_hot(top_i, N_EXPERTS, dtype=jnp.float32) * probs[..., None], axis=1)
    y = jnp.zeros((B * S, D), jnp.float32)
    for e in range(N_EXPERTS):
        y = y + combine[:, e:e + 1] * _swiglu(t, w_gate[e], w_up[e], w_down[e]).astype(jnp.float32)
    return y.reshape(B, S, D).astype(h.dtype)


def _trunk(x, c, ada_w, ada_b, norm1_g, norm2_g, w_in, pool_w, pool_scale, hgrn_lb, hgrn_norm_g,
           diff_lambda, diff_norm_g, w_out, ffn_w_gate, ffn_w_up, ffn_w_down, router_w, router_b,
           moe_w_gate, moe_w_up, moe_w_down, final_norm_g):
    p_lb = jax.nn.softmax(hgrn_lb.astype(jnp.float32), axis=0)
    lbs = jnp.cumsum(p_lb, axis=0) - p_lb[0:1]
    c_act = jax.nn.silu(c)
    for l in range(DEPTH):
        ada = (c_act @ ada_w[l] + ada_b[l])[:, None, :]
        sh1, sc1, g1, sh2, sc2, g2 = jnp.split(ada, 6, axis=-1)
        h = _rmsnorm(x, norm1_g[l]) * (1.0 + sc1) + sh1
        proj = h @ w_in[l]
        u, hq, hf_f, hf_b, hi, hg, dq, dk, dv = jnp.split(proj, SPLIT_POINTS, axis=-1)
        pool_out = _pool_mixer(u, pool_w[l], pool_scale[l])
        hgrn_out = _hgrn2_mixer(hq, hf_f, hf_b, hi, hg, lbs[l, 0], lbs[l, 1], hgrn_norm_g[l])
        lam_init = 0.8 - 0.6 * math.exp(-0.3 * l)
        lv = diff_lambda[l].astype(jnp.float32)
        lam = jnp.exp(jnp.sum(lv[0] * lv[1])) - jnp.exp(jnp.sum(lv[2] * lv[3])) + lam_init
        diff_out = _diff_attention(dq, dk, dv, lam, lam_init, diff_norm_g[l])
        mixed = jnp.concatenate([pool_out.astype(h.dtype), hgrn_out.astype(h.dtype), diff_out.astype(h.dtype)], axis=-1)
        x = x + g1 * (mixed @ w_out[l])
        h2 = _rmsnorm(x, norm2_g[l]) * (1.0 + sc2) + sh2
        if l % 2 == 0:
            j = l // 2
            f = _swiglu(h2, ffn_w_gate[j], ffn_w_up[j], ffn_w_down[j])
        else:
            j = l // 2
            f = _moe_ffn(h2, router_w[j], router_b[j], moe_w_gate[j], moe_w_up[j], moe_w_down[j])
        x = x + g2 * f
    return _rmsnorm(x, final_norm_g)


def setup_inputs(seed: int = 0) -> dict:
    key = jax.random.key(seed)
    ks = jax.random.split(key, 32)
    D = D_MODEL

    def nrm(k, shape, s):
        return jax.random.normal(k, shape, jnp.float32) * s

    return {
        'x_prompt': nrm(ks[0], (BATCH, SEQ, D), 1.0),
        'x_sample': nrm(ks[1], (DEC_BATCH, DEC_SEQ, D), 1.0),
        'c_prompt': nrm(ks[2], (BATCH, D), 1.0),
        'c_sample': nrm(ks[3], (DEC_BATCH, D), 1.0),
        'ada_w': nrm(ks[4], (DEPTH, D, 6 * D), 0.5 * D ** -0.5),
        'ada_b': nrm(ks[5], (DEPTH, 6 * D), 0.01),
        'norm1_g': 1.0 + nrm(ks[6], (DEPTH, D), 0.05),
        'norm2_g': 1.0 + nrm(ks[7], (DEPTH, D), 0.05),
        'w_in': nrm(ks[8], (DEPTH, D, IN_WIDTH), D ** -0.5),
        'pool_w': nrm(ks[9], (DEPTH, POOL_GROUPS, POOL_GROUP_DIM, POOL_GROUP_DIM), POOL_GROUP_DIM ** -0.5),
        'pool_scale': 1.0 + nrm(ks[10], (DEPTH, POOL_WIDTH), 0.1),
        'hgrn_lb': nrm(ks[11], (DEPTH, 2, HGRN_WIDTH), 0.5),
        'hgrn_norm_g': 1.0 + nrm(ks[12], (DEPTH, HGRN_HEAD_DIM), 0.05),
        'diff_lambda': nrm(ks[13], (DEPTH, 4, DIFF_HEAD_DIM), 0.1),
        'diff_norm_g': 1.0 + nrm(ks[14], (DEPTH, DIFF_V_DIM), 0.05),
        'w_out': nrm(ks[15], (DEPTH, D_MIX, D), D_MIX ** -0.5),
        'ffn_w_gate': nrm(ks[16], (N_DENSE, D, D_FF), D ** -0.5),
        'ffn_w_up': nrm(ks[17], (N_DENSE, D, D_FF), D ** -0.5),
        'ffn_w_down': nrm(ks[18], (N_DENSE, D_FF, D), D_FF ** -0.5),
        'router_w': nrm(ks[19], (N_MOE, D, N_EXPERTS), D ** -0.5),
        'router_b': nrm(ks[20], (N_MOE, N_EXPERTS), 0.01),
        'moe_w_gate': nrm(ks[21], (N_MOE, N_EXPERTS, D, D_FF_EXPERT), D ** -0.5),
        'moe_w_up': nrm(ks[22], (N_MOE, N_EXPERTS, D, D_FF_EXPERT), D ** -0.5),
        'moe_w_down': nrm(ks[23], (N_MOE, N_EXPERTS, D_FF_EXPERT, D), D_FF_EXPERT ** -0.5),
        'final_norm_g': 1.0 + nrm(ks[24], (D,), 0.05),
    }


def reference(x_prompt, x_sample, c_prompt, c_sample, ada_w, ada_b, norm1_g, norm2_g, w_in, pool_w,
              pool_scale, hgrn_lb, hgrn_norm_g, diff_lambda, diff_norm_g, w_out, ffn_w_gate, ffn_w_up,
              ffn_w_down, router_w, router_b, moe_w_gate, moe_w_up, moe_w_down, final_norm_g):
    y_prompt = _trunk(x_prompt, c_prompt, ada_w, ada_b, norm1_g, norm2_g, w_in, pool_w, pool_scale,
                      hgrn_lb, hgrn_norm_g, diff_lambda, diff_norm_g, w_out, ffn_w_gate, ffn_w_up,
                      ffn_w_down, router_w, router_b, moe_w_gate, moe_w_up, moe_w_down, final_norm_g)
    y_sample = _trunk(x_sample, c_sample, ada_w, ada_b, norm1_g, norm2_g, w_in, pool_w, pool_scale,
                      hgrn_lb, hgrn_norm_g, diff_lambda, diff_norm_g, w_out, ffn_w_gate, ffn_w_up,
                      ffn_w_down, router_w, router_b, moe_w_gate, moe_w_up, moe_w_down, final_norm_g)
    return (y_prompt, y_sample)
```

```python
import contextlib
import math
import os
import numpy as np
import ml_dtypes
import concourse.bass as bass
import concourse.mybir as mybir
from concourse.bass_utils import run_bass_kernel_spmd

F32 = mybir.dt.float32
BF16 = mybir.dt.bfloat16
ALU = mybir.AluOpType
AF = mybir.ActivationFunctionType
AX = mybir.AxisListType

D = 1024
DEPTH = 4
DFF = 2816
NE = 8
NJ = DFF // 128
EPS = 1e-6
BIG = 256.0
SAFE = os.environ.get('KSAFE', '1') == '1'
ATT_THR = 64.0


class Sched:
    def __init__(self, nc, stack):
        self.nc = nc
        self.units = {}
        self.waited = {}
        self.lastw = {}
        self.readers = {}
        self.stack = stack
        self.rr = {"sync": 0, "gpsimd": 0}
        self.nbar = 0
        self.bsemA = stack.enter_context(nc.semaphore("bsemA"))
        self.bsemB = stack.enter_context(nc.semaphore("bsemB"))

    def add_unit(self, name, eng, stream, inc, inorder):
        sem = self.stack.enter_context(self.nc.semaphore("sem_" + name))
        self.units[name] = dict(eng=eng, stream=stream, sem=sem, inc=inc, cnt=0, inorder=inorder)

    def _wait(self, stream, un, seq):
        key = (stream, un)
        if self.waited.get(key, 0) < seq:
            u = self.units[un]
            if not u["inorder"]:
                seq = u["cnt"]
            getattr(self.nc, stream).wait_ge(u["sem"], seq * u["inc"])
            self.waited[key] = seq

    def op(self, unit, fn, reads=(), writes=()):
        u = self.units[unit]
        need = {}
        for k in reads:
            for wun, wseq in self.lastw.get(k, {}).items():
                need[wun] = max(need.get(wun, 0), wseq)
        for k in writes:
            for wun, wseq in self.lastw.get(k, {}).items():
                need[wun] = max(need.get(wun, 0), wseq)
            for run, rseq in self.readers.get(k, {}).items():
                if run == unit and u["inorder"]:
                    continue
                need[run] = max(need.get(run, 0), rseq)
        if SAFE:
            for un, uu in self.units.items():
                if uu["cnt"] > 0 and not (un == unit and unit == "pe"):
                    self._wait(u["stream"], un, uu["cnt"])
        else:
            for un, seq in need.items():
                if un == unit and unit == "pe":
                    continue
                self._wait(u["stream"], un, seq)
        ins = fn(u["eng"])
        u["cnt"] += 1
        ins.then_inc(u["sem"], u["inc"])
        seq = u["cnt"]
        for k in writes:
            self.lastw.setdefault(k, {})[unit] = seq
        for k in reads:
            self.readers.setdefault(k, {})[unit] = seq
        return ins

    def dma(self, q, out, in_, reads=(), writes=()):
        q = "sync"
        return self.op(f"d_{q}", lambda e: e.dma_start(out=out, in_=in_), reads, writes)

    def barrier(self):
        streams = ("tensor", "scalar", "vector", "gpsimd", "sync")
        for stream in streams:
            for un, u in self.units.items():
                if u["cnt"] > 0:
                    self._wait(stream, un, u["cnt"])
        self.nbar += 1
        for stream in streams:
            getattr(self.nc, stream).sem_inc(self.bsemA, 1)
        self.nc.sync.wait_ge(self.bsemA, len(streams) * self.nbar)
        for un, u in self.units.items():
            if un != "d_gpsimd":
                self.nc.sync.sem_clear(u["sem"])
        self.nc.sync.sem_inc(self.bsemB, 1)
        for stream in streams:
            if stream != "sync":
                getattr(self.nc, stream).wait_ge(self.bsemB, self.nbar)
        for u in self.units.values():
            u["cnt"] = 0
        self.waited = {}
        self.lastw = {}
        self.readers = {}

    def checkpoint(self):
        if any(u["cnt"] * u["inc"] > 50000 for u in self.units.values()):
            self.barrier()


def build(T, L):
    SEG = T // 4
    NB = T // 512
    BPS = SEG // 512
    NKB = T // 128
    nc = bass.Bass("TRN2", target_bir_lowering=False)
    stack = contextlib.ExitStack()

    decl = {}

    def din(name, shape, dt=F32):
        decl[name] = (tuple(shape), dt)
        return nc.dram_tensor(name, list(shape), dt, kind="ExternalInput").ap()

    def dscr(name, shape, dt=F32):
        return nc.dram_tensor(name, list(shape), dt).ap()

    x_in = din("x", [T, D])
    y_out = nc.dram_tensor("y", [T, D], F32, kind="ExternalOutput").ap()
    cT_in = din("cT", [128, 8, 4])
    ada_w = din("ada_w", [L, D, 6 * D])
    ada_bT = din("ada_bT", [128, L, 48])
    n1T = din("n1T", [128, L, 8])
    n2T = din("n2T", [128, L, 8])
    nfT = din("nfT", [128, 8])
    w_in = din("w_in", [L, D, 3072])
    pool_w = din("pool_w", [L, 4, 64, 64])
    pool_scT = din("pool_scT", [128, L, 2])
    lbT = din("lbT", [128, L, 2, 2])
    hnT = din("hnT", [128, L])
    lam_b = din("lam_b", [128, L, 4, 64])
    dnT = din("dnT", [128, L])
    w_out = din("w_out", [L, D, D])
    ffn_g = din("ffn_g", [(L + 1) // 2, D, DFF])
    ffn_u = din("ffn_u", [(L + 1) // 2, D, DFF])
    ffn_d = din("ffn_d", [(L + 1) // 2, DFF, D])
    if L >= 2:
        rw = din("router_w", [max(L // 2, 1), D, NE])
        rbb = din("router_bb", [128, max(L // 2, 1), NE])
        moe_g = din("moe_g", [max(L // 2, 1), NE, D, DFF])
        moe_u = din("moe_u", [max(L // 2, 1), NE, D, DFF])
        moe_d = din("moe_d", [max(L // 2, 1), NE, DFF, D])
    ident_in = din("ident", [128, 128])
    jmat_in = din("jmat", [128, 128])
    segflag_in = din("segflag", [128, 1])
    invcnt_in = din("invcnt", [2, 128, T])
    kaug_in = din("kaug", [4, 9, T], BF16)
    qaugp_in = din("qaugp", [4, 9, T], BF16)
    qaugm_in = din("qaugm", [4, 9, T], BF16)
    dmat_in = din("dmat", [128, 4, 128], BF16)

    xT = dscr("s_xT", [D, T])
    uT = dscr("s_uT", [256, T], BF16)
    hqT = dscr("s_hqT", [2, 256, T], BF16)
    hvT = dscr("s_hvT", [2, 256, T], BF16)
    hgT = dscr("s_hgT", [256, T], BF16)
    ffT = dscr("s_ffT", [2, 256, T])
    kkT = dscr("s_kkT", [2, 256, T])
    qTd = dscr("s_qT", [4, 128, T], BF16)
    kTd = dscr("s_kT", [4, 128, T], BF16)
    Vd = dscr("s_V", [T, 512], BF16)
    if os.environ.get("KDBG"):
        mixT = nc.dram_tensor("dbg_mixT", [D, T], BF16, kind="ExternalOutput").ap()
    else:
        mixT = dscr("s_mixT", [D, T], BF16)
    NEa = NE if L >= 2 else 1
    wgu_s = dscr("s_wgu", [NEa, NJ, 128, 2, 8, 128], BF16)
    wd_s = dscr("s_wd", [NEa, 8, 128, NJ, 128], BF16)

    oT_all = dscr("s_oT", [2, 256, T])
    S = Sched(nc, stack)
    dbgx = nc.dram_tensor("dbg_x", [3, D, 512], F32, kind="ExternalOutput").ap() if os.environ.get("KDBG") else None

    def dump_x(i):
        if dbgx is not None:
            S.dma("sync", dbgx[i], xT[:, 0:512], reads=[("xT", 0)], writes=[("dbgx", i)])
            S.barrier()
    S.add_unit("pe", nc.tensor, "tensor", 1, True)
    S.add_unit("act", nc.scalar, "scalar", 1, True)
    S.add_unit("dve", nc.vector, "vector", 1, True)
    S.add_unit("pool", nc.gpsimd, "gpsimd", 1, True)
    S.add_unit("d_sync", nc.sync, "sync", 16, False)
    S.add_unit("d_gpsimd", nc.gpsimd, "gpsimd", 16, False)

    uid = [0]

    def sb(st, name, shape, dt=F32):
        uid[0] += 1
        return st.enter_context(nc.sbuf_tensor(f"t{uid[0]}_{name}", list(shape), dt))

    ps = [stack.enter_context(nc.psum_tensor(f"ps{i}", [128, 512], F32)) for i in range(8)]

    identF = sb(stack, "identF", [128, 128])
    identB = sb(stack, "identB", [128, 128], BF16)
    jF = sb(stack, "jF", [128, 128])
    jB = sb(stack, "jB", [128, 128], BF16)
    onesB = sb(stack, "onesB", [128, 128], BF16)
    blkonesB = sb(stack, "blkonesB", [128, 128], BF16)
    gsel = sb(stack, "gsel", [128, 192], BF16)
    segflag = sb(stack, "segflag", [128, 1])
    epsc = sb(stack, "epsc", [128, 1])
    adaT = sb(stack, "adaT", [128, L, 48, 4])
    A1 = sb(stack, "A1", [128, L, 8, 4])
    A2 = sb(stack, "A2", [128, L, 8, 4])
    n1s = sb(stack, "n1s", [128, L, 8])
    n2s = sb(stack, "n2s", [128, L, 8])
    nfs = sb(stack, "nfs", [128, 8])
    lbs = sb(stack, "lbs", [128, L, 2, 2])
    oml = sb(stack, "oml", [128, L, 2, 2])
    hns = sb(stack, "hns", [128, L])
    dns = sb(stack, "dns", [128, L])
    dns2 = sb(stack, "dns2", [128, L])
    lamc = sb(stack, "lamc", [128, L])
    pscs = sb(stack, "pscs", [128, L, 2])
    rbs = sb(stack, "rbs", [128, max(L // 2, 1), NE])
    dmat = sb(stack, "dmat", [128, 4, 128], BF16)

    S.dma("sync", identF[:], ident_in[:, :], writes=["identF"])
    S.dma("sync", jF[:], jmat_in[:, :], writes=["jF"])
    S.dma("sync", segflag[:], segflag_in[:, :], writes=["segflag"])
    S.dma("sync", n1s[:], n1T[:, :, :], writes=["n1s"])
    S.dma("sync", n2s[:], n2T[:, :, :], writes=["n2s"])
    S.dma("sync", nfs[:], nfT[:, :], writes=["nfs"])
    S.dma("sync", lbs[:], lbT[:, :, :, :], writes=["lbs"])
    S.dma("sync", hns[:], hnT[:, :], writes=["hns"])
    S.dma("sync", dns[:], dnT[:, :], writes=["dns"])
    S.dma("sync", pscs[:], pool_scT[:, :, :], writes=["pscs"])
    if L >= 2:
        S.dma("sync", rbs[:], rbb[:, :, :], writes=["rbs"])
    S.dma("sync", dmat[:], dmat_in[:, :, :], writes=["dmat"])
    S.op("dve", lambda e: e.tensor_copy(out=identB[:], in_=identF[:]), ["identF"], ["identB"])
    S.op("dve", lambda e: e.tensor_copy(out=jB[:], in_=jF[:]), ["jF"], ["jB"])
    S.op("dve", lambda e: e.memset(onesB[:], 1.0), [], ["onesB"])
    S.op("dve", lambda e: e.memset(blkonesB[:], 0.0), [], ["blkonesB"])
    S.op("dve", lambda e: e.memset(blkonesB[0:64, 0:64], 1.0), [], ["blkonesB"])
    S.op("dve", lambda e: e.memset(blkonesB[64:128, 64:128], 1.0), [], ["blkonesB"])
    S.op("dve", lambda e: e.memset(gsel[:], 0.0), [], ["gsel"])
    S.op("dve", lambda e: e.memset(gsel[0:64, 63:64], 1.0), [], ["gsel"])
    S.op("dve", lambda e: e.memset(gsel[64:128, 127:128], 1.0), [], ["gsel"])
    S.op("dve", lambda e: e.memset(epsc[:], EPS), [], ["epsc"])

    with contextlib.ExitStack() as st:
        cact = sb(st, "cact", [128, 8, 4])
        lamt = sb(st, "lamt", [128, L, 4, 64])
        lamp = sb(st, "lamp", [128, L, 2, 64])
        lams = sb(st, "lams", [128, L, 2])
        lbe = sb(st, "lbe", [128, L, 2, 2])
        lbsum = sb(st, "lbsum", [128, 2, 2])
        abT = sb(st, "abT", [128, L, 48])
        S.dma("sync", cact[:], cT_in[:, :, :], writes=["cact"])
        S.dma("sync", lamt[:], lam_b[:, :, :, :], writes=["lamt"])
        S.dma("sync", abT[:], ada_bT[:, :, :], writes=["abT"])
        S.op("act", lambda e: e.activation(out=cact[:], in_=cact[:], func=AF.Silu), ["cact"], ["cact"])
        for l in range(L):
            for j in range(2):
                S.op("dve", lambda e, l=l, j=j: e.tensor_tensor(out=lamp[:, l, j, :], in0=lamt[:, l, 2 * j, :],
                                                                in1=lamt[:, l, 2 * j + 1, :], op=ALU.mult),
                     ["lamt"], ["lamp"])
                S.op("dve", lambda e, l=l, j=j: e.reduce_sum(out=lams[:, l, j:j + 1], in_=lamp[:, l, j, :], axis=AX.X),
                     ["lamp"], ["lams"])
        S.op("act", lambda e: e.activation(out=lams[:], in_=lams[:], func=AF.Exp), ["lams"], ["lams"])
        for l in range(L):
            lam_init = 0.8 - 0.6 * math.exp(-0.3 * l)
            S.op("dve", lambda e, l=l: e.tensor_tensor(out=lamc[:, l:l + 1], in0=lams[:, l, 1:2], in1=lams[:, l, 0:1],
                                                       op=ALU.subtract), ["lams"], ["lamc"])
            S.op("dve", lambda e, l=l, li=lam_init: e.tensor_scalar(out=lamc[:, l:l + 1], in0=lamc[:, l:l + 1],
                                                                    scalar1=-li, scalar2=None, op0=ALU.add),
                 ["lamc"], ["lamc"])
            S.op("dve", lambda e, l=l, li=lam_init: e.tensor_scalar(out=dns2[:, l:l + 1], in0=dns[:, l:l + 1],
                                                                    scalar1=1.0 - li, scalar2=None, op0=ALU.mult),
                 ["dns"], ["dns2"])
        S.op("act", lambda e: e.activation(out=lbe[:], in_=lbs[:], func=AF.Exp), ["lbs"], ["lbe"])
        S.op("dve", lambda e: e.tensor_copy(out=lbsum[:], in_=lbe[:, 0]), ["lbe"], ["lbsum"])
        for l in range(1, L):
            S.op("dve", lambda e, l=l: e.tensor_tensor(out=lbsum[:], in0=lbsum[:], in1=lbe[:, l], op=ALU.add),
                 ["lbe", "lbsum"], ["lbsum"])
        S.op("dve", lambda e: e.reciprocal(out=lbsum[:], in_=lbsum[:]), ["lbsum"], ["lbsum"])
        for l in range(L):
            S.op("dve", lambda e, l=l: e.tensor_tensor(out=lbe[:, l], in0=lbe[:, l], in1=lbsum[:], op=ALU.mult),
                 ["lbe", "lbsum"], ["lbe"])
        S.op("dve", lambda e: e.memset(lbs[:, 0], 0.0), [], ["lbs"])
        for l in range(1, L):
            S.op("dve", lambda e, l=l: e.tensor_tensor(out=lbs[:, l], in0=lbs[:, l - 1], in1=lbe[:, l], op=ALU.add),
                 ["lbs", "lbe"], ["lbs"])
        S.op("dve", lambda e: e.tensor_scalar(out=oml[:], in0=lbs[:], scalar1=-1.0, scalar2=1.0, op0=ALU.mult,
                                              op1=ALU.add), ["lbs"], ["oml"])
        awt = [sb(st, f"awt{i}", [128, 8, 512]) for i in range(2)]
        it = 0
        for l in range(L):
            for g in range(12):
                buf = awt[it % 2]
                key = f"awt{it % 2}"
                S.dma("sync" if it % 2 == 0 else "gpsimd", buf[:],
                      ada_w[l, :, g * 512:(g + 1) * 512].rearrange("(k p) n -> p k n", p=128), writes=[key])
                for j in range(4):
                    oc = g * 4 + j
                    pst = ps[oc % 2]
                    for kc in range(8):
                        S.op("pe", lambda e, kc=kc, j=j, pst=pst, buf=buf: e.matmul(
                            pst[:, 0:4], lhsT=buf[:, kc, j * 128:(j + 1) * 128], rhs=cact[:, kc, :],
                            start=(kc == 0), stop=(kc == 7)), [key, "cact"], [f"ps{oc % 2}"])
                    S.op("dve", lambda e, l=l, oc=oc, pst=pst: e.tensor_scalar(
                        out=adaT[:, l, oc, :], in0=pst[:, 0:4], scalar1=abT[:, l, oc:oc + 1], scalar2=None,
                        op0=ALU.add), [f"ps{oc % 2}", "abT"], ["adaT"])
                it += 1
        for l in range(L):
            for c in range(8):
                S.op("dve", lambda e, l=l, c=c: e.tensor_scalar(
                    out=A1[:, l, c, :], in0=adaT[:, l, 8 + c, :], scalar1=1.0, scalar2=n1s[:, l, c:c + 1],
                    op0=ALU.add, op1=ALU.mult), ["adaT", "n1s"], ["A1"])
                S.op("dve", lambda e, l=l, c=c: e.tensor_scalar(
                    out=A2[:, l, c, :], in0=adaT[:, l, 32 + c, :], scalar1=1.0, scalar2=n2s[:, l, c:c + 1],
                    op0=ALU.add, op1=ALU.mult), ["adaT", "n2s"], ["A2"])
        S.barrier()

    with contextlib.ExitStack() as st:
        xin = [sb(st, f"xin{i}", [128, 4, D]) for i in range(2)]
        xo = [sb(st, f"xo{i}", [128, 8, 512]) for i in range(2)]
        for b in range(NB):
            xi, xk = xin[b % 2], f"xin{b % 2}"
            S.dma("sync", xi[:], x_in[b * 512:(b + 1) * 512, :].rearrange("(j p) d -> p j d", p=128), writes=[xk])
            for c in range(8):
                for j in range(4):
                    S.op("pe", lambda e, c=c, j=j, xi=xi: e.transpose(
                        ps[c][:, j * 128:(j + 1) * 128], xi[:, j, c * 128:(c + 1) * 128], identF[:]),
                        [xk, "identF"], [f"ps{c}"])
                eng = "act" if c % 2 else "dve"
                if eng == "act":
                    S.op("act", lambda e, c=c, b=b: e.copy(out=xo[b % 2][:, c, :], in_=ps[c][:]), [f"ps{c}"],
                         [f"xo{b % 2}"])
                else:
                    S.op("dve", lambda e, c=c, b=b: e.tensor_copy(out=xo[b % 2][:, c, :], in_=ps[c][:]), [f"ps{c}"],
                         [f"xo{b % 2}"])
            S.dma("gpsimd", xT[:, b * 512:(b + 1) * 512].rearrange("(c p) t -> p c t", p=128), xo[b % 2][:],
                  reads=[f"xo{b % 2}"], writes=[("xT", b)])
        S.barrier()

    dump_x(0)
    def norm_block(st_tiles, l, Acoef, shift_base, b, want_f32=False):
        xt, sq, rstd, tmp, hT, hF = st_tiles
        seg = b // BPS
        for c in range(8):
            S.op("act", lambda e, c=c: e.activation(out=sq[:, c, :], in_=xt[:, c, :], func=AF.Square), ["xt"],
                 ["sq"])
        for c in range(8):
            S.op("pe", lambda e, c=c: e.matmul(ps[7][:], lhsT=onesB[:], rhs=sq[:, c, :], start=(c == 0),
                                               stop=(c == 7)), ["sq", "onesB"], ["ps7"])
        S.op("act", lambda e: e.activation(out=rstd[:], in_=ps[7][:], func=AF.Sqrt, bias=epsc[:, 0:1],
                                           scale=1.0 / D), ["ps7", "epsc"], ["rstd"])
        S.op("dve", lambda e: e.reciprocal(out=rstd[:], in_=rstd[:]), ["rstd"], ["rstd"])
        for c in range(8):
            S.op("dve", lambda e, c=c: e.scalar_tensor_tensor(
                out=tmp[:, c % 2, :], in0=xt[:, c, :], scalar=Acoef[:, l, c, seg:seg + 1], in1=rstd[:],
                op0=ALU.mult, op1=ALU.mult), ["xt", "rstd", "A1", "A2"], [f"tmp{c % 2}"])
            if want_f32:
                S.op("act", lambda e, c=c: e.activation(
                    out=hF[:, c, :], in_=tmp[:, c % 2, :], func=AF.Identity,
                    bias=adaT[:, l, shift_base + c, seg:seg + 1], scale=1.0), [f"tmp{c % 2}", "adaT"], ["hF"])
                S.op("dve", lambda e, c=c: e.tensor_copy(out=hT[:, c, :], in_=hF[:, c, :]), ["hF"], ["hT"])
            else:
                S.op("act", lambda e, c=c: e.activation(
                    out=hT[:, c, :], in_=tmp[:, c % 2, :], func=AF.Identity,
                    bias=adaT[:, l, shift_base + c, seg:seg + 1], scale=1.0), [f"tmp{c % 2}", "adaT"], ["hT"])

    def cast_weights_kmajor(st, src_ap, ncols, dst, dkey, stage, skey):
        for kc in range(8):
            S.dma("sync", stage[:, 0:ncols], src_ap[kc * 128:(kc + 1) * 128, :], writes=[skey])
            if kc % 2 == 0:
                S.op("act", lambda e, kc=kc: e.copy(out=dst[:, kc, :], in_=stage[:, 0:ncols]), [skey], [dkey])
            else:
                S.op("dve", lambda e, kc=kc: e.tensor_copy(out=dst[:, kc, :], in_=stage[:, 0:ncols]), [skey], [dkey])

    def prep_ffn_weights(wg_ap, wu_ap, wd_ap, e_idx):
        with contextlib.ExitStack() as st:
            stg = [sb(st, f"pw_stg{i}", [128, DFF]) for i in range(2)]
            wb = [sb(st, f"pw_b{i}", [128, DFF], BF16) for i in range(2)]
            it = 0
            for which, wap in ((0, wg_ap), (1, wu_ap)):
                for kc in range(8):
                    i = it % 2
                    S.dma("sync", stg[i][:], wap[kc * 128:(kc + 1) * 128, :], writes=[f"pw_stg{i}"])
                    if i == 0:
                        S.op("act", lambda e, i=i: e.copy(out=wb[i][:], in_=stg[i][:]), [f"pw_stg{i}"], [f"pw_b{i}"])
                    else:
                        S.op("dve", lambda e, i=i: e.tensor_copy(out=wb[i][:], in_=stg[i][:]), [f"pw_stg{i}"],
                             [f"pw_b{i}"])
                    S.dma("gpsimd", wgu_s[e_idx, :, :, which, kc, :].rearrange("j p n -> p j n"),
                          wb[i][:].rearrange("p (j n) -> p j n", n=128), reads=[f"pw_b{i}"],
                          writes=[("wgu", e_idx)])
                    it += 1
            for j in range(NJ):
                i = it % 2
                S.dma("sync", stg[i][:, 0:D], wd_ap[j * 128:(j + 1) * 128, :], writes=[f"pw_stg{i}"])
                if i == 0:
                    S.op("act", lambda e, i=i: e.copy(out=wb[i][:, 0:D], in_=stg[i][:, 0:D]), [f"pw_stg{i}"],
                         [f"pw_b{i}"])
                else:
                    S.op("dve", lambda e, i=i: e.tensor_copy(out=wb[i][:, 0:D], in_=stg[i][:, 0:D]), [f"pw_stg{i}"],
                         [f"pw_b{i}"])
                S.dma("gpsimd", wd_s[e_idx, :, :, j, :].rearrange("o p n -> p o n"),
                      wb[i][:, 0:D].rearrange("p (o n) -> p o n", n=128), reads=[f"pw_b{i}"],
                      writes=[("wd", e_idx)])
                it += 1
            S.barrier()

    slopes = [2.0 ** (-8.0 * (h + 1) / 4) for h in range(4)]

    for l in range(L):
        is_moe = (l % 2 == 1)
        jl = l // 2
        PH = os.environ.get('KPH', '12345')
        with contextlib.ExitStack() as st:
          if '1' in PH:
            Win = sb(st, "Win", [128, 8, 3072], BF16)
            stage = sb(st, "wstage", [128, 3072])
            cast_weights_kmajor(st, w_in[l], 3072, Win, "Win", stage, "wstage")
            xt = sb(st, "xt", [128, 8, 512])
            sq = sb(st, "sq", [128, 8, 512], BF16)
            rstd = sb(st, "rstd", [128, 512])
            tmp = sb(st, "tmp", [128, 2, 512])
            hT = sb(st, "hT", [128, 8, 512], BF16)
            ev = [sb(st, f"ev{i}", [128, 512]) for i in range(4)]
            evb = [sb(st, f"evb{i}", [128, 512], BF16) for i in range(4)]
            evf = [sb(st, f"evf{i}", [128, 512]) for i in range(4)]
            tiles = (xt, sq, rstd, tmp, hT, None)
            n_ev = 0
            for b in range(NB):
                S.checkpoint()
                tsl = slice(b * 512, (b + 1) * 512)
                S.dma("sync", xt[:], xT[:, tsl].rearrange("(c p) t -> p c t", p=128), reads=[("xT", b)],
                      writes=["xt"])
                norm_block(tiles, l, A1, 0, b)
                for oc in range(20):
                    pi = oc % 4
                    pst, pk = ps[pi], f"ps{pi}"
                    for kc in range(8):
                        S.op("pe", lambda e, kc=kc, oc=oc, pst=pst: e.matmul(
                            pst[:], lhsT=Win[:, kc, oc * 128:(oc + 1) * 128], rhs=hT[:, kc, :], start=(kc == 0),
                            stop=(kc == 7)), ["Win", "hT"], [pk])
                    i = n_ev % 4
                    n_ev += 1
                    eb, ebk = evb[i], f"evb{i}"
                    if oc < 2:
                        S.op("act", lambda e, pst=pst, eb=eb: e.copy(out=eb[:], in_=pst[:]), [pk], [ebk])
                        S.dma("gpsimd", uT[oc * 128:(oc + 1) * 128, tsl], eb[:], reads=[ebk], writes=[("uT", b)])
                    elif oc < 4:
                        S.op("act", lambda e, pst=pst, eb=eb: e.activation(out=eb[:], in_=pst[:], func=AF.Silu), [pk],
                             [ebk])
                        S.dma("gpsimd", hqT[0, (oc - 2) * 128:(oc - 1) * 128, tsl], eb[:], reads=[ebk],
                              writes=[("hqT", b)])
                    elif oc < 8:
                        d_ = (oc - 4) // 2
                        hp = (oc - 4) % 2
                        e1, e1k = ev[i], f"ev{i}"
                        e2, e2k = evf[i], f"evf{i}"
                        S.op("act", lambda e, pst=pst, e1=e1: e.activation(out=e1[:], in_=pst[:], func=AF.Sigmoid),
                             [pk], [e1k])
                        S.op("dve", lambda e, e1=e1, d_=d_, hp=hp: e.tensor_scalar(
                            out=e1[:], in0=e1[:], scalar1=oml[:, l, d_, hp:hp + 1], scalar2=lbs[:, l, d_, hp:hp + 1],
                            op0=ALU.mult, op1=ALU.add), [e1k, "oml", "lbs"], [e1k])
                        S.op("dve", lambda e, e1=e1, e2=e2: e.tensor_scalar(
                            out=e2[:], in0=e1[:], scalar1=-1.0, scalar2=1.0, op0=ALU.mult, op1=ALU.add), [e1k], [e2k])
                        if d_ == 0:
                            S.dma("gpsimd", ffT[0, hp * 128:(hp + 1) * 128, tsl], e1[:], reads=[e1k],
                                  writes=[("ffT0", b)])
                            S.dma("gpsimd", kkT[0, hp * 128:(hp + 1) * 128, tsl], e2[:], reads=[e2k],
                                  writes=[("kkT0", b)])
                        else:
                            rb = NB - 1 - b
                            rsl = slice(rb * 512, (rb + 1) * 512)
                            for src, sk, dst_ap, dk in ((e1, e1k, ffT, "ffT1"), (e2, e2k, kkT, "kkT1")):
                                for j in range(4):
                                    S.op("pe", lambda e, src=src, j=j: e.transpose(
                                        ps[6][:, j * 128:(j + 1) * 128], src[:, j * 128:(j + 1) * 128], identF[:]),
                                        [sk, "identF"], ["ps6"])
                                S.op("act", lambda e, src=src: e.copy(out=src[:], in_=ps[6][:]), ["ps6"], [sk])
                                for j in range(4):
                                    S.op("pe", lambda e, src=src, j=j: e.matmul(
                                        ps[6][:, (3 - j) * 128:(4 - j) * 128], lhsT=src[:, j * 128:(j + 1) * 128],
                                        rhs=jF[:], start=True, stop=True), [sk, "jF"], ["ps6"])
                                S.op("act", lambda e, src=src: e.copy(out=src[:], in_=ps[6][:]), ["ps6"], [sk])
                                S.dma("gpsimd", dst_ap[1, hp * 128:(hp + 1) * 128, rsl], src[:], reads=[sk],
                                      writes=[(dk, rb)])
                    elif oc < 10:
                        S.op("act", lambda e, pst=pst, eb=eb: e.copy(out=eb[:], in_=pst[:]), [pk], [ebk])
                        S.dma("gpsimd", hvT[0, (oc - 8) * 128:(oc - 7) * 128, tsl], eb[:], reads=[ebk],
                              writes=[("hvT", b)])
                    elif oc < 12:
                        S.op("act", lambda e, pst=pst, eb=eb: e.activation(out=eb[:], in_=pst[:], func=AF.Silu), [pk],
                             [ebk])
                        S.dma("gpsimd", hgT[(oc - 10) * 128:(oc - 9) * 128, tsl], eb[:], reads=[ebk],
                              writes=[("hgT", b)])
                    elif oc < 16:
                        S.op("act", lambda e, pst=pst, eb=eb: e.activation(out=eb[:], in_=pst[:], func=AF.Copy,
                                                                           scale=0.125), [pk], [ebk])
                        S.dma("gpsimd", qTd[oc - 12, :, tsl], eb[:], reads=[ebk], writes=[("qTd", b)])
                    else:
                        S.op("act", lambda e, pst=pst, eb=eb: e.copy(out=eb[:], in_=pst[:]), [pk], [ebk])
                        S.dma("gpsimd", kTd[oc - 16, :, tsl], eb[:], reads=[ebk], writes=[("kTd", b)])
                    if oc in (2, 3, 8, 9):
                        rb = NB - 1 - b
                        rsl = slice(rb * 512, (rb + 1) * 512)
                        for j in range(4):
                            S.op("pe", lambda e, eb=eb, j=j: e.matmul(
                                ps[6][:, j * 128:(j + 1) * 128], lhsT=eb[:, j * 128:(j + 1) * 128], rhs=identB[:],
                                start=True, stop=True), [ebk, "identB"], ["ps6"])
                        i2 = n_ev % 4
                        n_ev += 1
                        eb2, eb2k = evb[i2], f"evb{i2}"
                        S.op("act", lambda e, eb2=eb2: e.copy(out=eb2[:], in_=ps[6][:]), ["ps6"], [eb2k])
                        for j in range(4):
                            S.op("pe", lambda e, eb2=eb2, j=j: e.matmul(
                                ps[6][:, (3 - j) * 128:(4 - j) * 128], lhsT=eb2[:, j * 128:(j + 1) * 128], rhs=jB[:],
                                start=True, stop=True), [eb2k, "jB"], ["ps6"])
                        S.op("act", lambda e, eb2=eb2: e.copy(out=eb2[:], in_=ps[6][:]), ["ps6"], [eb2k])
                        if oc < 4:
                            S.dma("gpsimd", hqT[1, (oc - 2) * 128:(oc - 1) * 128, rsl], eb2[:], reads=[eb2k],
                                  writes=[("hqT1", rb)])
                        else:
                            S.dma("gpsimd", hvT[1, (oc - 8) * 128:(oc - 7) * 128, rsl], eb2[:], reads=[eb2k],
                                  writes=[("hvT1", rb)])
                for j in range(4):
                    pi = j % 4
                    pst, pk = ps[pi], f"ps{pi}"
                    for kc in range(8):
                        S.op("pe", lambda e, kc=kc, j=j, pst=pst: e.matmul(
                            pst[:], lhsT=hT[:, kc, j * 128:(j + 1) * 128], rhs=Win[:, kc, 2560:3072],
                            start=(kc == 0), stop=(kc == 7)), ["Win", "hT"], [pk])
                    i = n_ev % 4
                    n_ev += 1
                    eb, ebk = evb[i], f"evb{i}"
                    S.op("dve", lambda e, pst=pst, eb=eb: e.tensor_copy(out=eb[:], in_=pst[:]), [pk], [ebk])
                    S.dma("gpsimd", Vd[b * 512 + j * 128:b * 512 + (j + 1) * 128, :], eb[:], reads=[ebk],
                          writes=[("Vd", b)])
            S.barrier()

        with contextlib.ExitStack() as st:
          if '2' in PH:
            PB = 512
            up = sb(st, "up", [128, PB + 32], BF16)
            uf = sb(st, "uf", [128, PB + 32])
            pa = sb(st, "pa", [128, PB + 32])
            pb_ = sb(st, "pb", [128, PB + 32])
            icn = sb(st, "icn", [128, PB])
            dd = sb(st, "dd", [128, PB], BF16)
            pw = sb(st, "pw", [128, 2, 128], BF16)
            pwf = sb(st, "pwf", [128, 2, 128])
            mo = sb(st, "mo", [128, PB], BF16)
            S.op("dve", lambda e: e.memset(pwf[:], 0.0), [], ["pwf"])
            for g in range(4):
                c, hh = g // 2, g % 2
                S.dma("sync", pwf[hh * 64:(hh + 1) * 64, c, hh * 64:(hh + 1) * 64], pool_w[l, g, :, :],
                      writes=["pwf"])
            S.op("dve", lambda e: e.tensor_copy(out=pw[:], in_=pwf[:]), ["pwf"], ["pw"])
            sh = {0: (1, 3), 1: (3, 7)}
            for c in range(2):
                for b in range(NB):
                    S.checkpoint()
                    t0 = b * PB
                    rk = [("uT", bb) for bb in range(max(b - 1, 0), min(b + 2, NB))]
                    lo = 16 if b == 0 else 0
                    hi = PB + 16 if b == NB - 1 else PB + 32
                    if b == 0:
                        S.op("dve", lambda e: e.memset(up[:, 0:16], 0.0), [], ["up"])
                    if b == NB - 1:
                        S.op("dve", lambda e: e.memset(up[:, PB + 16:PB + 32], 0.0), [], ["up"])
                    S.dma("sync", up[:, lo:hi], uT[c * 128:(c + 1) * 128, t0 - 16 + lo:t0 - 16 + hi], reads=rk,
                          writes=["up"])
                    S.dma("sync", icn[:], invcnt_in[c, :, t0:t0 + PB], writes=["icn"])
                    S.op("dve", lambda e: e.tensor_copy(out=uf[:], in_=up[:]), ["up"], ["uf"])
                    if b % BPS == 0 and b > 0:
                        S.op("dve", lambda e: e.tensor_scalar(out=uf[:, 0:16], in0=uf[:, 0:16],
                                                              scalar1=segflag[:, 0:1], scalar2=None, op0=ALU.mult),
                             ["uf", "segflag"], ["uf"])
                    if b % BPS == BPS - 1 and b < NB - 1:
                        S.op("dve", lambda e: e.tensor_scalar(out=uf[:, PB + 16:PB + 32], in0=uf[:, PB + 16:PB + 32],
                                                              scalar1=segflag[:, 0:1], scalar2=None, op0=ALU.mult),
                             ["uf", "segflag"], ["uf"])
                    W = PB + 32
                    S.op("dve", lambda e: e.tensor_tensor(out=pa[:, 1:W], in0=uf[:, 1:W], in1=uf[:, 0:W - 1],
                                                          op=ALU.add), ["uf"], ["pa"])
                    S.op("dve", lambda e: e.tensor_tensor(out=pb_[:, 3:W], in0=pa[:, 3:W], in1=pa[:, 1:W - 2],
                                                          op=ALU.add), ["pa"], ["pb"])
                    if c == 0:
                        S.op("dve", lambda e: e.tensor_tensor(out=pa[0:64, 16:16 + PB], in0=pa[0:64, 16:16 + PB],
                                                              in1=icn[0:64, :], op=ALU.mult), ["pa", "icn"], ["pa"])
                        S.op("dve", lambda e: e.tensor_tensor(out=pa[64:128, 16:16 + PB], in0=pb_[64:128, 17:17 + PB],
                                                              in1=icn[64:128, :], op=ALU.mult), ["pb", "pa", "icn"],
                             ["pa"])
                        S.op("dve", lambda e: e.tensor_tensor(out=dd[:], in0=pa[:, 16:16 + PB], in1=uf[:, 16:16 + PB],
                                                              op=ALU.subtract), ["pa", "uf"], ["dd"])
                    else:
                        S.op("dve", lambda e: e.tensor_tensor(out=pa[:, 7:W], in0=pb_[:, 7:W], in1=pb_[:, 3:W - 4],
                                                              op=ALU.add), ["pb"], ["pa"])
                        S.op("dve", lambda e: e.tensor_tensor(out=pb_[64:128, 15:W], in0=pa[64:128, 15:W],
                                                              in1=pa[64:128, 7:W - 8], op=ALU.add), ["pa"],
                             ["pb"])
                        S.op("dve", lambda e: e.tensor_tensor(out=pb_[0:64, 16:16 + PB], in0=pa[0:64, 19:19 + PB],
                                                              in1=icn[0:64, :], op=ALU.mult), ["pa", "pb", "icn"],
                             ["pb"])
                        S.op("dve", lambda e: e.tensor_tensor(out=pa[64:128, 16:16 + PB], in0=pb_[64:128, 23:23 + PB],
                                                              in1=icn[64:128, :], op=ALU.mult), ["pb", "pa", "icn"],
                             ["pa"])
                        S.op("dve", lambda e: e.tensor_tensor(out=dd[0:64, :], in0=pb_[0:64, 16:16 + PB],
                                                              in1=uf[0:64, 16:16 + PB], op=ALU.subtract),
                             ["pb", "uf"], ["dd"])
                        S.op("dve", lambda e: e.tensor_tensor(out=dd[64:128, :], in0=pa[64:128, 16:16 + PB],
                                                              in1=uf[64:128, 16:16 + PB], op=ALU.subtract),
                             ["pa", "uf"], ["dd"])
                    S.op("pe", lambda e, c=c: e.matmul(ps[0][:], lhsT=pw[:, c, :], rhs=dd[:], start=True, stop=True),
                         ["pw", "dd"], ["ps0"])
                    S.op("act", lambda e, c=c: e.activation(out=mo[:], in_=ps[0][:], func=AF.Copy,
                                                            scale=pscs[:, l, c:c + 1]), ["ps0", "pscs"], ["mo"])
                    S.dma("gpsimd", mixT[c * 128:(c + 1) * 128, t0:t0 + PB], mo[:], reads=["mo"],
                          writes=[("mixT", b)])
            S.barrier()

        with contextlib.ExitStack() as st:
          if '3' in PH:
            TBH = min(2048, T)
            NTB = T // TBH
            f_t = sb(st, "f_t", [128, TBH])
            k_t = sb(st, "k_t", [128, TBH])
            q_t = sb(st, "q_t", [128, TBH], BF16)
            vb = [sb(st, f"vb{i}", [128, TBH], BF16) for i in range(2)]
            d1 = sb(st, "d1", [128, TBH])
            sc = sb(st, "sc", [128, TBH])
            pr = [sb(st, f"pr{i}", [128, TBH], BF16) for i in range(2)]
            state = sb(st, "state", [128, 64])
            oacc = sb(st, "oacc", [128, 512])
            orev = sb(st, "orev", [128, 512])
            osq = sb(st, "osq", [128, 512], BF16)
            orstd = sb(st, "orstd", [128, 512])
            og = sb(st, "og", [128, 512], BF16)
            oo = sb(st, "oo", [128, 512], BF16)
            oT = oT_all
            for hp in range(2):
                for d_ in range(2):
                    S.op("dve", lambda e: e.memset(state[:], 0.0), [], ["state"])
                    for tb in range(NTB):
                        tsl = slice(tb * TBH, (tb + 1) * TBH)
                        blks = range(tb * TBH // 512, (tb + 1) * TBH // 512)
                        S.dma("sync", f_t[:], ffT[d_, hp * 128:(hp + 1) * 128, tsl],
                              reads=[(f"ffT{d_}", bb) for bb in blks], writes=["f_t"])
                        S.dma("sync", k_t[:], kkT[d_, hp * 128:(hp + 1) * 128, tsl],
                              reads=[(f"kkT{d_}", bb) for bb in blks], writes=["k_t"])
                        S.dma("sync", q_t[:], hqT[d_, hp * 128:(hp + 1) * 128, tsl],
                              reads=[("hqT" if d_ == 0 else "hqT1", bb) for bb in blks], writes=["q_t"])
                        for s_ in range(1, 4):
                            tt = s_ * SEG
                            if tb * TBH <= tt < (tb + 1) * TBH:
                                o_ = tt - tb * TBH
                                S.op("dve", lambda e, o_=o_: e.tensor_scalar(
                                    out=f_t[:, o_:o_ + 1], in0=f_t[:, o_:o_ + 1], scalar1=segflag[:, 0:1],
                                    scalar2=None, op0=ALU.mult), ["f_t", "segflag"], ["f_t"])
                        for ee in range(64):
                            S.checkpoint() if ee % 16 == 0 else None
                            i = ee % 2
                            for hh in range(2):
                                row = hp * 128 + hh * 64 + ee
                                S.dma("sync" if hh == 0 else "gpsimd", vb[i][hh * 64:(hh + 1) * 64, :],
                                      hvT[d_, row:row + 1, tsl].broadcast_to([64, TBH]),
                                      reads=[("hvT" if d_ == 0 else "hvT1", bb) for bb in blks], writes=[f"vb{i}"])
                            S.op("dve", lambda e, i=i: e.tensor_tensor(out=d1[:], in0=k_t[:], in1=vb[i][:],
                                                                       op=ALU.mult), ["k_t", f"vb{i}"], ["d1"])
                            S.op("dve", lambda e, ee=ee: e.tensor_tensor_scan(
                                out=sc[:], data0=f_t[:], data1=d1[:], initial=state[:, ee:ee + 1], op0=ALU.mult,
                                op1=ALU.add), ["f_t", "d1", "state"], ["sc"])
                            S.op("act", lambda e, ee=ee: e.copy(out=state[:, ee:ee + 1], in_=sc[:, TBH - 1:TBH]),
                                 ["sc"], ["state"])
                            S.op("dve", lambda e, i=i: e.tensor_tensor(out=pr[i][:], in0=sc[:], in1=q_t[:],
                                                                       op=ALU.mult), ["sc", "q_t"], [f"pr{i}"])
                            for jb in range(TBH // 512):
                                S.op("pe", lambda e, ee=ee, jb=jb, i=i: e.matmul(
                                    ps[jb][:], lhsT=gsel[:, 63 - ee:63 - ee + 128], rhs=pr[i][:, jb * 512:(jb + 1) * 512],
                                    start=(ee == 0), stop=(ee == 63)), [f"pr{i}", "gsel"], [f"ps{jb}"])
                        for jb in range(TBH // 512):
                            gb = tb * (TBH // 512) + jb
                            if d_ == 0:
                                S.op("act", lambda e, jb=jb: e.copy(out=oacc[:], in_=ps[jb][:]), [f"ps{jb}"], ["oacc"])
                                S.dma("gpsimd", oT[0, hp * 128:(hp + 1) * 128, gb * 512:(gb + 1) * 512], oacc[:],
                                      reads=["oacc"], writes=[("oT0", gb)])
                            else:
                                rb = NB - 1 - gb
                                S.op("act", lambda e, jb=jb: e.copy(out=orev[:], in_=ps[jb][:]), [f"ps{jb}"], ["orev"])
                                for j in range(4):
                                    S.op("pe", lambda e, j=j: e.transpose(
                                        ps[6][:, j * 128:(j + 1) * 128], orev[:, j * 128:(j + 1) * 128], identF[:]),
                                        ["orev", "identF"], ["ps6"])
                                S.op("act", lambda e: e.copy(out=orev[:], in_=ps[6][:]), ["ps6"], ["orev"])
                                for j in range(4):
                                    S.op("pe", lambda e, j=j: e.matmul(
                                        ps[6][:, (3 - j) * 128:(4 - j) * 128], lhsT=orev[:, j * 128:(j + 1) * 128],
                                        rhs=jF[:], start=True, stop=True), ["orev", "jF"], ["ps6"])
                                S.op("act", lambda e: e.copy(out=orev[:], in_=ps[6][:]), ["ps6"], ["orev"])
                                S.dma("gpsimd", oT[1, hp * 128:(hp + 1) * 128, rb * 512:(rb + 1) * 512], orev[:],
                                      reads=["orev"], writes=[("oT1", rb)])
                for b in range(NB):
                    tsl = slice(b * 512, (b + 1) * 512)
                    S.dma("sync", oacc[:], oT[0, hp * 128:(hp + 1) * 128, tsl], reads=[("oT0", b)], writes=["oacc"])
                    S.dma("sync", orev[:], oT[1, hp * 128:(hp + 1) * 128, tsl], reads=[("oT1", b)], writes=["orev"])
                    S.dma("sync", og[:], hgT[hp * 128:(hp + 1) * 128, tsl], reads=[("hgT", b)], writes=["og"])
                    S.op("dve", lambda e: e.tensor_tensor(out=oacc[:], in0=oacc[:], in1=orev[:], op=ALU.add),
                         ["oacc", "orev"], ["oacc"])
                    S.op("act", lambda e: e.activation(out=osq[:], in_=oacc[:], func=AF.Square), ["oacc"], ["osq"])
                    S.op("pe", lambda e: e.matmul(ps[7][:], lhsT=blkonesB[:], rhs=osq[:], start=True, stop=True),
                         ["osq", "blkonesB"], ["ps7"])
                    S.op("act", lambda e: e.activation(out=orstd[:], in_=ps[7][:], func=AF.Sqrt, bias=epsc[:, 0:1],
                                                       scale=1.0 / 64), ["ps7", "epsc"], ["orstd"])
                    S.op("dve", lambda e: e.reciprocal(out=orstd[:], in_=orstd[:]), ["orstd"], ["orstd"])
                    S.op("dve", lambda e: e.scalar_tensor_tensor(out=oacc[:], in0=oacc[:], scalar=hns[:, l:l + 1],
                                                                 in1=orstd[:], op0=ALU.mult, op1=ALU.mult),
                         ["oacc", "orstd", "hns"], ["oacc"])
                    S.op("dve", lambda e: e.tensor_tensor(out=oo[:], in0=oacc[:], in1=og[:], op=ALU.mult),
                         ["oacc", "og"], ["oo"])
                    S.dma("gpsimd", mixT[256 + hp * 128:256 + (hp + 1) * 128, tsl], oo[:], reads=["oo"],
                          writes=[("mixT", b)])
            S.barrier()

        with contextlib.ExitStack() as st:
          if '4' in PH:
            KA = [sb(st, f"KA{m}", [128, T], BF16) for m in range(2)]
            Vh = sb(st, "Vh", [128, NKB, 128], BF16)
            Qp = [sb(st, f"Qp{m}", [128, 512], BF16) for m in range(2)]
            Qm = [sb(st, f"Qm{m}", [128, 512], BF16) for m in range(2)]
            PT = [sb(st, f"PT{i}", [128, 512], BF16) for i in range(4)]
            r0 = sb(st, "r0", [128, 512])
            r1 = sb(st, "r1", [128, 512])
            ao = sb(st, "ao", [128, 512])
            asq = sb(st, "asq", [128, 512], BF16)
            arstd = sb(st, "arstd", [128, 512])
            aob = sb(st, "aob", [128, 512], BF16)
            KR = 73
            for h in range(4):
                slope = slopes[h]
                dskip = ATT_THR / slope
                for m in range(2):
                    S.dma("sync", KA[m][0:64, :], kTd[h, m * 64:(m + 1) * 64, :],
                          reads=[("kTd", bb) for bb in range(NB)], writes=[f"KA{m}"])
                    S.dma("gpsimd", KA[m][64:73, :], kaug_in[h, :, :], writes=[f"KA{m}"])
                S.dma("sync", Vh[:], Vd[:, h * 128:(h + 1) * 128].rearrange("(n p) d -> p n d", p=128),
                      reads=[("Vd", bb) for bb in range(NB)], writes=["Vh"])
                npt = 0
                for qc in range(NB):
                    S.checkpoint()
                    qsl = slice(qc * 512, (qc + 1) * 512)
                    for m in range(2):
                        S.dma("sync", Qp[m][0:64, :], qTd[h, m * 64:(m + 1) * 64, qsl], reads=[("qTd", qc)],
                              writes=[f"Qp{m}"])
                        S.dma("sync", Qp[m][64:73, :], qaugp_in[h, :, qsl], writes=[f"Qp{m}"])
                        S.dma("gpsimd", Qm[m][0:64, :], qTd[h, m * 64:(m + 1) * 64, qsl], reads=[("qTd", qc)],
                              writes=[f"Qm{m}"])
                        S.dma("gpsimd", Qm[m][64:73, :], qaugm_in[h, :, qsl], writes=[f"Qm{m}"])
                    kbs = []
                    for kb in range(NKB):
                        k0, k1 = kb * 128, kb * 128 + 127
                        q0, q1 = qc * 512, qc * 512 + 511
                        if k1 < q0:
                            dist = q0 - k1
                        elif k0 > q1:
                            dist = k0 - q1
                        else:
                            dist = 0
                        if dist < dskip:
                            kbs.append(kb)
                    for ik, kb in enumerate(kbs):
                        first, last = (ik == 0), (ik == len(kbs) - 1)
                        ksl = slice(kb * 128, (kb + 1) * 128)
                        for m in range(2):
                            pi = 4 + (npt % 3)
                            pst, pk = ps[pi], f"ps{pi}"
                            jj = kb - qc * 4
                            if jj < 0:
                                S.op("pe", lambda e, m=m, pst=pst, ksl=ksl: e.matmul(
                                    pst[:], lhsT=KA[m][0:KR, ksl], rhs=Qp[m][0:KR, :], start=True, stop=True),
                                    [f"KA{m}", f"Qp{m}"], [pk])
                            elif jj > 3:
                                S.op("pe", lambda e, m=m, pst=pst, ksl=ksl: e.matmul(
                                    pst[:], lhsT=KA[m][0:KR, ksl], rhs=Qm[m][0:KR, :], start=True, stop=True),
                                    [f"KA{m}", f"Qm{m}"], [pk])
                            else:
                                for s_ in range(4):
                                    csl = slice(s_ * 128, (s_ + 1) * 128)
                                    if s_ < jj:
                                        S.op("pe", lambda e, m=m, pst=pst, ksl=ksl, csl=csl: e.matmul(
                                            pst[:, csl], lhsT=KA[m][0:KR, ksl], rhs=Qm[m][0:KR, csl], start=True,
                                            stop=True), [f"KA{m}", f"Qm{m}"], [pk])
                                    elif s_ > jj:
                                        S.op("pe", lambda e, m=m, pst=pst, ksl=ksl, csl=csl: e.matmul(
                                            pst[:, csl], lhsT=KA[m][0:KR, ksl], rhs=Qp[m][0:KR, csl], start=True,
                                            stop=True), [f"KA{m}", f"Qp{m}"], [pk])
                                    else:
                                        S.op("pe", lambda e, m=m, pst=pst, ksl=ksl, csl=csl: e.matmul(
                                            pst[:, csl], lhsT=KA[m][0:64, ksl], rhs=Qp[m][0:64, csl], start=True,
                                            stop=False), [f"KA{m}", f"Qp{m}"], [pk])
                                        S.op("pe", lambda e, pst=pst, csl=csl, h=h: e.matmul(
                                            pst[:, csl], lhsT=identB[:], rhs=dmat[:, h, :], start=False, stop=True),
                                            ["identB", "dmat"], [pk])
                            pti = npt % 4
                            npt += 1
                            S.op("act", lambda e, pst=pst, pti=pti: e.activation(out=PT[pti][:], in_=pst[:],
                                                                                 func=AF.Exp), [pk], [f"PT{pti}"])
                            S.op("pe", lambda e, m=m, kb=kb, pti=pti: e.matmul(
                                ps[m][:], lhsT=Vh[:, kb, :], rhs=PT[pti][:], start=first, stop=last),
                                ["Vh", f"PT{pti}"], [f"ps{m}"])
                            S.op("pe", lambda e, m=m, pti=pti: e.matmul(
                                ps[2 + m][:], lhsT=onesB[:], rhs=PT[pti][:], start=first, stop=last),
                                ["onesB", f"PT{pti}"], [f"ps{2 + m}"])
                    S.op("dve", lambda e: e.reciprocal(out=r0[:], in_=ps[2][:]), ["ps2"], ["r0"])
                    S.op("dve", lambda e: e.reciprocal(out=r1[:], in_=ps[3][:]), ["ps3"], ["r1"])
                    S.op("dve", lambda e: e.tensor_tensor(out=r0[:], in0=r0[:], in1=ps[0][:], op=ALU.mult),
                         ["r0", "ps0"], ["r0"])
                    S.op("dve", lambda e: e.tensor_tensor(out=r1[:], in0=r1[:], in1=ps[1][:], op=ALU.mult),
                         ["r1", "ps1"], ["r1"])
                    S.op("dve", lambda e: e.scalar_tensor_tensor(out=ao[:], in0=r1[:], scalar=lamc[:, l:l + 1],
                                                                 in1=r0[:], op0=ALU.mult, op1=ALU.add),
                         ["r0", "r1", "lamc"], ["ao"])
                    S.op("act", lambda e: e.activation(out=asq[:], in_=ao[:], func=AF.Square), ["ao"], ["asq"])
                    S.op("pe", lambda e: e.matmul(ps[7][:], lhsT=onesB[:], rhs=asq[:], start=True, stop=True),
                         ["asq", "onesB"], ["ps7"])
                    S.op("act", lambda e: e.activation(out=arstd[:], in_=ps[7][:], func=AF.Sqrt, bias=epsc[:, 0:1],
                                                       scale=1.0 / 128), ["ps7", "epsc"], ["arstd"])
                    S.op("dve", lambda e: e.reciprocal(out=arstd[:], in_=arstd[:]), ["arstd"], ["arstd"])
                    S.op("dve", lambda e: e.scalar_tensor_tensor(out=aob[:], in0=ao[:], scalar=dns2[:, l:l + 1],
                                                                 in1=arstd[:], op0=ALU.mult, op1=ALU.mult),
                         ["ao", "arstd", "dns2"], ["aob"])
                    S.dma("gpsimd", mixT[512 + h * 128:512 + (h + 1) * 128, qsl], aob[:], reads=["aob"],
                          writes=[("mixT", qc)])
            S.barrier()

        n_exp = NE if is_moe else 1
        for e_ in range(n_exp if '5' in PH else 0):
            if is_moe:
                prep_ffn_weights(moe_g[jl, e_], moe_u[jl, e_], moe_d[jl, e_], e_)
            else:
                prep_ffn_weights(ffn_g[jl], ffn_u[jl], ffn_d[jl], 0)
        with contextlib.ExitStack() as st:
          if '5' in PH:
            Wout = sb(st, "Wout", [128, 8, D], BF16)
            stage = sb(st, "wstage2", [128, D])
            cast_weights_kmajor(st, w_out[l], D, Wout, "Wout", stage, "wstage2")
            xt = sb(st, "xt", [128, 8, 512])
            sq = sb(st, "sq", [128, 8, 512], BF16)
            rstd = sb(st, "rstd", [128, 512])
            tmp = sb(st, "tmp", [128, 2, 512])
            hT = sb(st, "hT", [128, 8, 512], BF16)
            hF = sb(st, "hF", [128, 8, 512]) if is_moe else None
            mx = sb(st, "mx", [128, 8, 512], BF16)
            aT = sb(st, "aT", [128, NJ, 512], BF16)
            sg = [sb(st, f"sg{i}", [128, 512]) for i in range(2)]
            a2 = [sb(st, f"a2{i}", [128, 512]) for i in range(2)]
            wgu = [sb(st, f"wgu{i}", [128, 2, 8, 128], BF16) for i in range(3)]
            wdt = [sb(st, f"wdt{i}", [128, NJ, 128], BF16) for i in range(2)]
            yacc = sb(st, "yacc", [128, 8, 512]) if is_moe else None
            if is_moe:
                Wr = sb(st, "Wr", [128, 8, NE])
                lgT = sb(st, "lgT", [NE, 512])
                lg = sb(st, "lg", [128, 4, NE])
                top8 = sb(st, "top8", [128, 4, 8])
                msk = sb(st, "msk", [128, 4, NE])
                nm1 = sb(st, "nm1", [128, 4])
                den = sb(st, "den", [128, 4])
                combT = sb(st, "combT", [NE, 512], BF16)
                combTf = sb(st, "combTf", [NE, 512])
                selE = sb(st, "selE", [NE, NE, 128], BF16)
                combB = [sb(st, f"combB{i}", [128, 512]) for i in range(2)]
                S.dma("sync", Wr[:], rw[jl].rearrange("(k p) n -> p k n", p=128), writes=["Wr"])
                S.op("dve", lambda e: e.memset(selE[:], 0.0), [], ["selE"])
                for e_ in range(NE):
                    S.op("dve", lambda e, e_=e_: e.tensor_scalar(
                        out=selE[:, e_, :], in0=selE[:, e_, :], scalar1=identF[0:NE, e_:e_ + 1], scalar2=None,
                        op0=ALU.add), ["selE", "identF"], ["selE"])
            tiles = (xt, sq, rstd, tmp, hT, hF)
            nw = 0
            nd = 0
            for b in range(NB):
                S.checkpoint()
                tsl = slice(b * 512, (b + 1) * 512)
                seg = b // BPS
                S.dma("sync", xt[:], xT[:, tsl].rearrange("(c p) t -> p c t", p=128), reads=[("xT", b)], writes=["xt"])
                S.dma("gpsimd", mx[:], mixT[:, tsl].rearrange("(c p) t -> p c t", p=128), reads=[("mixT", b)],
                      writes=["mx"])
                for oc in range(8):
                    pi = oc % 2
                    for kc in range(8):
                        S.op("pe", lambda e, kc=kc, oc=oc, pi=pi: e.matmul(
                            ps[pi][:], lhsT=Wout[:, kc, oc * 128:(oc + 1) * 128], rhs=mx[:, kc, :], start=(kc == 0),
                            stop=(kc == 7)), ["Wout", "mx"], [f"ps{pi}"])
                    S.op("dve", lambda e, oc=oc, pi=pi: e.scalar_tensor_tensor(
                        out=xt[:, oc, :], in0=ps[pi][:], scalar=adaT[:, l, 16 + oc, seg:seg + 1], in1=xt[:, oc, :],
                        op0=ALU.mult, op1=ALU.add), [f"ps{pi}", "xt", "adaT"], ["xt"])
                norm_block(tiles, l, A2, 24, b, want_f32=is_moe)
                if is_moe:
                    for kc in range(8):
                        S.op("pe", lambda e, kc=kc: e.matmul(ps[6][0:NE, :], lhsT=Wr[:, kc, :], rhs=hF[:, kc, :],
                                                             start=(kc == 0), stop=(kc == 7)), ["Wr", "hF"], ["ps6"])
                    S.op("act", lambda e: e.copy(out=lgT[:], in_=ps[6][0:NE, :]), ["ps6"], ["lgT"])
                    for j in range(4):
                        S.op("pe", lambda e, j=j: e.transpose(ps[6][:, j * 8:(j + 1) * 8],
                                                              lgT[:, j * 128:(j + 1) * 128], identF[0:NE, 0:NE]),
                             ["lgT", "identF"], ["ps6"])
                    for j in range(4):
                        S.op("dve", lambda e, j=j: e.tensor_tensor(out=lg[:, j, :], in0=ps[6][:, j * 8:(j + 1) * 8],
                                                                   in1=rbs[:, jl, :], op=ALU.add), ["ps6", "rbs"],
                             ["lg"])
                        S.op("dve", lambda e, j=j: e.max(out=top8[:, j, :], in_=lg[:, j, :]), ["lg"], ["top8"])
                        S.op("dve", lambda e, j=j: e.tensor_scalar(out=msk[:, j, :], in0=lg[:, j, :],
                                                                   scalar1=top8[:, j, 1:2], scalar2=None,
                                                                   op0=ALU.is_ge), ["lg", "top8"], ["msk"])
                        S.op("dve", lambda e, j=j: e.tensor_scalar(out=nm1[:, j:j + 1], in0=top8[:, j, 0:1],
                                                                   scalar1=-1.0, scalar2=None, op0=ALU.mult),
                             ["top8"], ["nm1"])
                        S.op("act", lambda e, j=j: e.activation(out=lg[:, j, :], in_=lg[:, j, :], func=AF.Exp,
                                                                bias=nm1[:, j:j + 1], scale=1.0), ["lg", "nm1"],
                             ["lg"])
                        S.op("dve", lambda e, j=j: e.tensor_tensor(out=lg[:, j, :], in0=lg[:, j, :], in1=msk[:, j, :],
                                                                   op=ALU.mult), ["lg", "msk"], ["lg"])
                        S.op("dve", lambda e, j=j: e.reduce_sum(out=den[:, j:j + 1], in_=lg[:, j, :], axis=AX.X),
                             ["lg"], ["den"])
                        S.op("dve", lambda e, j=j: e.reciprocal(out=den[:, j:j + 1], in_=den[:, j:j + 1]), ["den"],
                             ["den"])
                        S.op("dve", lambda e, j=j: e.tensor_scalar(out=lg[:, j, :], in0=lg[:, j, :],
                                                                   scalar1=den[:, j:j + 1], scalar2=None,
                                                                   op0=ALU.mult), ["lg", "den"], ["lg"])
                        S.op("pe", lambda e, j=j: e.transpose(ps[5][0:NE, j * 128:(j + 1) * 128], lg[:, j, :],
                                                              identF[:]), ["lg", "identF"], ["ps5"])
                    S.op("act", lambda e: e.copy(out=combT[:], in_=ps[5][0:NE, :]), ["ps5"], ["combT"])
                for e_ in range(n_exp):
                    if is_moe:
                        ci = e_ % 2
                        S.op("pe", lambda e, e_=e_: e.matmul(ps[6][:], lhsT=selE[:, e_, :], rhs=combT[:], start=True,
                                                             stop=True), ["selE", "combT"], ["ps6"])
                        S.op("act", lambda e, ci=ci: e.copy(out=combB[ci][:], in_=ps[6][:]), ["ps6"], [f"combB{ci}"])
                    for j in range(NJ):
                        wi = nw % 3
                        nw += 1
                        S.dma("sync" if nw % 2 else "gpsimd", wgu[wi][:], wgu_s[e_, j], reads=[("wgu", e_)],
                              writes=[f"wgu{wi}"])
                        pg, pu = 2 + 2 * (j % 2), 3 + 2 * (j % 2)
                        for kc in range(8):
                            S.op("pe", lambda e, kc=kc, wi=wi, pg=pg: e.matmul(
                                ps[pg][:], lhsT=wgu[wi][:, 0, kc, :], rhs=hT[:, kc, :], start=(kc == 0),
                                stop=(kc == 7)), [f"wgu{wi}", "hT"], [f"ps{pg}"])
                        for kc in range(8):
                            S.op("pe", lambda e, kc=kc, wi=wi, pu=pu: e.matmul(
                                ps[pu][:], lhsT=wgu[wi][:, 1, kc, :], rhs=hT[:, kc, :], start=(kc == 0),
                                stop=(kc == 7)), [f"wgu{wi}", "hT"], [f"ps{pu}"])
                        si = j % 2
                        S.op("act", lambda e, si=si, pg=pg: e.activation(out=sg[si][:], in_=ps[pg][:], func=AF.Silu),
                             [f"ps{pg}"], [f"sg{si}"])
                        if is_moe:
                            S.op("dve", lambda e, si=si, pu=pu: e.tensor_tensor(out=a2[si][:], in0=sg[si][:],
                                                                                in1=ps[pu][:], op=ALU.mult),
                                 [f"sg{si}", f"ps{pu}"], [f"a2{si}"])
                            S.op("dve", lambda e, si=si, j=j, ci=ci: e.tensor_tensor(
                                out=aT[:, j, :], in0=a2[si][:], in1=combB[ci][:], op=ALU.mult),
                                [f"a2{si}", f"combB{ci}"], ["aT"])
                        else:
                            S.op("dve", lambda e, si=si, pu=pu, j=j: e.tensor_tensor(
                                out=aT[:, j, :], in0=sg[si][:], in1=ps[pu][:], op=ALU.mult),
                                [f"sg{si}", f"ps{pu}"], ["aT"])
                    for oc in range(8):
                        di = nd % 2
                        nd += 1
                        S.dma("sync" if nd % 2 else "gpsimd", wdt[di][:], wd_s[e_, oc], reads=[("wd", e_)],
                              writes=[f"wdt{di}"])
                        pi = oc % 2
                        for j in range(NJ):
                            S.op("pe", lambda e, j=j, di=di, pi=pi: e.matmul(
                                ps[pi][:], lhsT=wdt[di][:, j, :], rhs=aT[:, j, :], start=(j == 0),
                                stop=(j == NJ - 1)), [f"wdt{di}", "aT"], [f"ps{pi}"])
                        if not is_moe:
                            S.op("dve", lambda e, oc=oc, pi=pi: e.scalar_tensor_tensor(
                                out=xt[:, oc, :], in0=ps[pi][:], scalar=adaT[:, l, 40 + oc, seg:seg + 1],
                                in1=xt[:, oc, :], op0=ALU.mult, op1=ALU.add), [f"ps{pi}", "xt", "adaT"], ["xt"])
                        elif e_ == 0:
                            S.op("act", lambda e, oc=oc, pi=pi: e.copy(out=yacc[:, oc, :], in_=ps[pi][:]),
                                 [f"ps{pi}"], ["yacc"])
                        else:
                            S.op("dve", lambda e, oc=oc, pi=pi: e.tensor_tensor(
                                out=yacc[:, oc, :], in0=yacc[:, oc, :], in1=ps[pi][:], op=ALU.add),
                                [f"ps{pi}", "yacc"], ["yacc"])
                if is_moe:
                    for oc in range(8):
                        S.op("dve", lambda e, oc=oc: e.scalar_tensor_tensor(
                            out=xt[:, oc, :], in0=yacc[:, oc, :], scalar=adaT[:, l, 40 + oc, seg:seg + 1],
                            in1=xt[:, oc, :], op0=ALU.mult, op1=ALU.add), ["yacc", "xt", "adaT"], ["xt"])
                S.dma("gpsimd", xT[:, tsl].rearrange("(c p) t -> p c t", p=128), xt[:], reads=["xt"],
                      writes=[("xT", b)])
            S.barrier()

    dump_x(1)
    with contextlib.ExitStack() as st:
        xt = sb(st, "xt", [128, 8, 512])
        sq = sb(st, "sq", [128, 8, 512], BF16)
        rstd = sb(st, "rstd", [128, 512])
        yo = [sb(st, f"yo{i}", [128, 4, D]) for i in range(2)]
        for b in range(NB):
            tsl = slice(b * 512, (b + 1) * 512)
            S.dma("sync", xt[:], xT[:, tsl].rearrange("(c p) t -> p c t", p=128), reads=[("xT", b)], writes=["xt"])
            for c in range(8):
                S.op("act", lambda e, c=c: e.activation(out=sq[:, c, :], in_=xt[:, c, :], func=AF.Square), ["xt"],
                     ["sq"])
            for c in range(8):
                S.op("pe", lambda e, c=c: e.matmul(ps[7][:], lhsT=onesB[:], rhs=sq[:, c, :], start=(c == 0),
                                                   stop=(c == 7)), ["sq", "onesB"], ["ps7"])
            S.op("act", lambda e: e.activation(out=rstd[:], in_=ps[7][:], func=AF.Sqrt, bias=epsc[:, 0:1],
                                               scale=1.0 / D), ["ps7", "epsc"], ["rstd"])
            S.op("dve", lambda e: e.reciprocal(out=rstd[:], in_=rstd[:]), ["rstd"], ["rstd"])
            for c in range(8):
                S.op("dve", lambda e, c=c: e.scalar_tensor_tensor(
                    out=xt[:, c, :], in0=xt[:, c, :], scalar=nfs[:, c:c + 1], in1=rstd[:], op0=ALU.mult,
                    op1=ALU.mult), ["xt", "rstd", "nfs"], ["xt"])
            yi = b % 2
            for j in range(4):
                pi = j % 2
                for c in range(4):
                    S.op("pe", lambda e, c=c, j=j, pi=pi: e.transpose(
                        ps[pi][:, c * 128:(c + 1) * 128], xt[:, c, j * 128:(j + 1) * 128], identF[:]),
                        ["xt", "identF"], [f"ps{pi}"])
                S.op("act", lambda e, j=j, pi=pi, yi=yi: e.copy(out=yo[yi][:, j, 0:512], in_=ps[pi][:]), [f"ps{pi}"],
                     [f"yo{yi}"])
                for c in range(4):
                    S.op("pe", lambda e, c=c, j=j, pi=pi: e.transpose(
                        ps[2 + pi][:, c * 128:(c + 1) * 128], xt[:, 4 + c, j * 128:(j + 1) * 128], identF[:]),
                        ["xt", "identF"], [f"ps{2 + pi}"])
                S.op("dve", lambda e, j=j, pi=pi, yi=yi: e.tensor_copy(out=yo[yi][:, j, 512:1024], in_=ps[2 + pi][:]),
                     [f"ps{2 + pi}"], [f"yo{yi}"])
            S.dma("gpsimd", y_out[b * 512:(b + 1) * 512, :].rearrange("(j p) d -> p j d", p=128), yo[yi][:],
                  reads=[f"yo{yi}"], writes=[("y", b)])
        S.barrier()
    stack.close()
    nc._decl = decl
    nc._counts = {k: u['cnt'] for k, u in S.units.items()}
    return nc


def _role_tables(T, connected):
    SEG = T // 4
    t = np.arange(T)
    seg = t // SEG
    bf = ml_dtypes.bfloat16
    slopes = [2.0 ** (-8.0 * (h + 1) / 4) for h in range(4)]
    kaug = np.zeros((4, 9, T), np.float32)
    qp = np.zeros((4, 9, T), np.float32)
    qm = np.zeros((4, 9, T), np.float32)
    hi = (t // 128) * 128.0
    lo = (t % 128) * 1.0
    for h in range(4):
        s = slopes[h]
        kaug[h, 0] = s * hi
        kaug[h, 1] = s * lo
        kaug[h, 2] = 1.0
        kaug[h, 3] = 1.0
        kaug[h, 4] = 1.0
        qp[h, 0] = 1.0
        qp[h, 1] = 1.0
        qp[h, 2] = -s * hi
        qp[h, 3] = -s * lo
        qm[h, 0:4] = -qp[h, 0:4]
        for arr in (qp, qm):
            arr[h, 4] = -BIG
        for sgi in range(4):
            oh = (seg == sgi).astype(np.float32) if not connected else np.full(T, 0.25 * 0 + (1.0 if sgi == 0 else 0.0))
            kaug[h, 5 + sgi] = oh
            qp[h, 5 + sgi] = BIG * oh
            qm[h, 5 + sgi] = BIG * oh
    invc = np.zeros((2, 128, T), np.float32)
    wins = (2, 4, 8, 16)
    for g, w in enumerate(wins):
        if connected:
            lo_ = np.clip(t - w // 2, 0, T)
            hi_ = np.clip(t + w // 2, 0, T)
        else:
            tl = t % SEG
            lo_ = np.clip(tl - w // 2, 0, SEG)
            hi_ = np.clip(tl + w // 2, 0, SEG)
        ic = 1.0 / (hi_ - lo_).astype(np.float32)
        c, hh = g // 2, g % 2
        invc[c, hh * 64:(hh + 1) * 64, :] = ic[None, :]
    dm = np.zeros((128, 4, 128), np.float32)
    ii = np.arange(128)
    for h in range(4):
        dm[:, h, :] = -slopes[h] * np.abs(ii[:, None] - ii[None, :])
    return dict(kaug=kaug.astype(bf), qaugp=qp.astype(bf), qaugm=qm.astype(bf), invcnt=invc,
                dmat=dm.astype(bf), segflag=np.full((128, 1), 1.0 if connected else 0.0, np.float32))


_CACHE = {}


def _run(jobs_x, jobs_c, connected, params, T, L):
    if (T, L) not in _CACHE:
        _CACHE[(T, L)] = build(T, L)
    nc = _CACHE[(T, L)]
    p = params
    f32 = np.float32

    def colT(a, nchunk):
        a = np.asarray(a, f32)
        lead = a.shape[:-1]
        return np.ascontiguousarray(np.moveaxis(a.reshape(lead + (nchunk, 128)), -1, 0))

    shared = dict(
        ada_w=np.ascontiguousarray(p["ada_w"][:L], f32),
        ada_bT=colT(p["ada_b"][:L], 48),
        n1T=colT(p["norm1_g"][:L], 8), n2T=colT(p["norm2_g"][:L], 8), nfT=colT(p["final_norm_g"], 8),
        w_in=np.ascontiguousarray(p["w_in"][:L], f32),
        pool_w=np.ascontiguousarray(p["pool_w"][:L], f32),
        pool_scT=colT(p["pool_scale"][:L], 2),
        lbT=colT(p["hgrn_lb"], 2),
        hnT=np.ascontiguousarray(np.tile(np.asarray(p["hgrn_norm_g"][:L], f32).T, (2, 1))),
        lam_b=np.ascontiguousarray(np.broadcast_to(np.asarray(p["diff_lambda"][:L], f32)[None], (128, L, 4, 64))),
        dnT=np.ascontiguousarray(np.asarray(p["diff_norm_g"][:L], f32).T),
        w_out=np.ascontiguousarray(p["w_out"][:L], f32),
        ffn_g=np.ascontiguousarray(p["ffn_w_gate"][:(L + 1) // 2], f32),
        ffn_u=np.ascontiguousarray(p["ffn_w_up"][:(L + 1) // 2], f32),
        ffn_d=np.ascontiguousarray(p["ffn_w_down"][:(L + 1) // 2], f32),
        router_w=np.ascontiguousarray(p["router_w"][:max(L // 2, 1)], f32),
        router_bb=np.ascontiguousarray(np.broadcast_to(np.asarray(p["router_b"][:max(L // 2, 1)], f32)[None],
                                                       (128, max(L // 2, 1), NE))),
        moe_g=np.ascontiguousarray(p["moe_w_gate"][:max(L // 2, 1)], f32),
        moe_u=np.ascontiguousarray(p["moe_w_up"][:max(L // 2, 1)], f32),
        moe_d=np.ascontiguousarray(p["moe_w_down"][:max(L // 2, 1)], f32),
        ident=np.eye(128, dtype=f32), jmat=np.ascontiguousarray(np.eye(128, dtype=f32)[::-1]),
    )
    if L < 2:
        for k in ("router_w", "router_bb", "moe_g", "moe_u", "moe_d"):
            shared.pop(k)
    roles = {True: _role_tables(T, True), False: _role_tables(T, False)}
    in_maps = []
    for c in range(8):
        m = dict(shared)
        m.update(roles[connected[c]])
        m["x"] = np.ascontiguousarray(jobs_x[c], f32)
        m["cT"] = np.ascontiguousarray(np.asarray(jobs_c[c], f32).T.reshape(8, 128, 4).transpose(1, 0, 2))
        in_maps.append(m)
    for k, (shp, dt) in nc._decl.items():
        a = in_maps[0].get(k)
        if a is None or tuple(a.shape) != shp or (a.dtype == np.float32) != (dt == F32):
            print("DECL MISMATCH", k, shp, dt, None if a is None else (a.shape, a.dtype), flush=True)
    for k in in_maps[0]:
        if k not in nc._decl:
            print("EXTRA INPUT", k, flush=True)
    res = run_bass_kernel_spmd(nc, in_maps, core_ids=list(range(8)))
    if os.environ.get("KDBG"):
        _CACHE["dbg"] = [res.results[c]["dbg_mixT"] for c in range(2)]
        _CACHE["dbgx"] = [res.results[c]["dbg_x"] for c in range(2)]
        _CACHE["jobs_x"] = jobs_x
    return [res.results[c]["y"] for c in range(8)]


def kernel(x_prompt, x_sample, c_prompt, c_sample, **params):
    T = 16384
    xp = np.asarray(x_prompt, np.float32)
    xs = np.asarray(x_sample, np.float32)
    cp = np.asarray(c_prompt, np.float32)
    cs = np.asarray(c_sample, np.float32)
    jobs_x, jobs_c, conn = [], [], []
    for b in range(2):
        jobs_x.append(xp[b])
        jobs_c.append(np.repeat(cp[b:b + 1], 4, axis=0))
        conn.append(True)
    for j in range(4):
        jobs_x.append(xs[4 * j:4 * j + 4].reshape(T, D))
        jobs_c.append(cs[4 * j:4 * j + 4])
        conn.append(False)
    for b in range(2):
        jobs_x.append(jobs_x[b])
        jobs_c.append(jobs_c[b])
        conn.append(True)
    ys = _run(jobs_x, jobs_c, conn, params, T, DEPTH)
    y_prompt = np.stack([ys[0], ys[1]], axis=0).astype(np.float32)
    y_sample = np.concatenate([ys[2 + j].reshape(4, 4096, D) for j in range(4)], axis=0).astype(np.float32)
    return (y_prompt, y_sample)
```
